# Optimizing a Trainium2 kernel written in Bass

```python
import jax, jax.numpy as jnp
from jax import lax
import numpy as np

D_MODEL = 1024
BATCH = 8
SEQ = 4096
DEPTH = 2

CHUNK = 64
Q_BLOCK = 2 * CHUNK
CONV_WIDTH = 3
D_CONV = D_MODEL // 2
N_HEADS = 8
HEAD_DIM = 64
D_ATTN = N_HEADS * HEAD_DIM
D_FF = 2816
N_EXPERTS = 8
TOP_K = 2
D_FF_EXPERT = 2048
N_DENSE = (DEPTH + 1) // 2
N_MOE = DEPTH // 2
EPS = 1e-6
SPLITS = np.cumsum([D_CONV, D_CONV, D_CONV, D_ATTN, D_ATTN, D_ATTN, D_MODEL]).tolist()
D_IN_PROJ = 3 * D_CONV + 3 * D_ATTN + 2 * D_MODEL

kernel_name = "hybrid_shortconv_stickbreaking_moe_block"


def rmsnorm(x, g):
    xf = x.astype(jnp.float32)
    xf = xf * lax.rsqrt(jnp.mean(xf * xf, axis=-1, keepdims=True) + EPS)
    return (xf * g.astype(jnp.float32)).astype(x.dtype)


def causal_dwconv(u, w):
    s = u.shape[1]
    up = jnp.pad(u, ((0, 0), (CONV_WIDTH - 1, 0), (0, 0)))
    return sum(w[k] * up[:, k:k + s] for k in range(CONV_WIDTH))


def stick_breaking_attention(q, k, v):
    b, s, h, dh = q.shape
    n_blk = s // Q_BLOCK
    scale = dh ** -0.5
    qb = q.reshape(b, n_blk, Q_BLOCK, h, dh).transpose(1, 0, 3, 2, 4)
    kk = k.transpose(0, 2, 1, 3)
    vv = v.transpose(0, 2, 1, 3)
    key_pos = jnp.arange(s)

    def one_block(args):
        qi, blk = args
        q_pos = blk * Q_BLOCK + jnp.arange(Q_BLOCK)
        mask = key_pos[None, :] < q_pos[:, None]
        z = jnp.einsum('bhqd,bhkd->bhqk', qi, kk).astype(jnp.float32) * scale
        log_beta = jax.nn.log_sigmoid(z)
        log_1m = jnp.where(mask, log_beta - z, 0.0)
        incl = lax.cumsum(log_1m, axis=3, reverse=True)
        excl = jnp.concatenate([incl[..., 1:], jnp.zeros_like(incl[..., :1])], axis=-1)
        a = jnp.where(mask, jnp.exp(log_beta + excl), 0.0)
        return jnp.einsum('bhqk,bhkd->bhqd', a.astype(v.dtype), vv)

    o = lax.map(one_block, (qb, jnp.arange(n_blk)))
    return o.transpose(1, 0, 3, 2, 4).reshape(b, s, h * dh)


def swiglu(h, wg, wu, wd):
    return (jax.nn.silu(h @ wg) * (h @ wu)) @ wd


def moe_swiglu(h, w_router, wg, wu, wd):
    logits = h.astype(jnp.float32) @ w_router.astype(jnp.float32)
    top_vals, top_idx = lax.top_k(logits, TOP_K)
    top_w = jax.nn.softmax(top_vals, axis=-1)
    combine = jnp.sum(jax.nn.one_hot(top_idx, N_EXPERTS, dtype=jnp.float32) * top_w[..., None], axis=-2)
    combine = combine.astype(h.dtype)
    out = jnp.zeros_like(h)
    for e in range(N_EXPERTS):
        out = out + combine[..., e:e + 1] * swiglu(h, wg[e], wu[e], wd[e])
    return out


def setup_inputs(seed: int = 0) -> dict:
    key = jax.random.key(seed)
    ks = jax.random.split(key, 17)

    def nrm(k, shape, fan_in):
        return jax.random.normal(k, shape, jnp.float32) * (fan_in ** -0.5)

    def gain(k, shape):
        return 1.0 + 0.02 * jax.random.normal(k, shape, jnp.float32)

    return {
        "x": jax.random.normal(ks[0], (BATCH, SEQ, D_MODEL), jnp.float32),
        "g_mix": gain(ks[1], (DEPTH, D_MODEL)),
        "w_in": nrm(ks[2], (DEPTH, D_MODEL, D_IN_PROJ), D_MODEL),
        "conv_w": nrm(ks[3], (DEPTH, CONV_WIDTH, D_CONV), CONV_WIDTH),
        "w_branch_conv": nrm(ks[4], (DEPTH, D_CONV, D_MODEL), D_CONV),
        "w_branch_attn": nrm(ks[5], (DEPTH, D_ATTN, D_MODEL), D_ATTN),
        "w_out": nrm(ks[6], (DEPTH, D_MODEL, D_MODEL), D_MODEL),
        "g_ffn": gain(ks[7], (DEPTH, D_MODEL)),
        "w_ffn_gate": nrm(ks[8], (N_DENSE, D_MODEL, D_FF), D_MODEL),
        "w_ffn_up": nrm(ks[9], (N_DENSE, D_MODEL, D_FF), D_MODEL),
        "w_ffn_down": nrm(ks[10], (N_DENSE, D_FF, D_MODEL), D_FF),
        "w_router": nrm(ks[11], (N_MOE, D_MODEL, N_EXPERTS), D_MODEL),
        "w_exp_gate": nrm(ks[12], (N_MOE, N_EXPERTS, D_MODEL, D_FF_EXPERT), D_MODEL),
        "w_exp_up": nrm(ks[13], (N_MOE, N_EXPERTS, D_MODEL, D_FF_EXPERT), D_MODEL),
        "w_exp_down": nrm(ks[14], (N_MOE, N_EXPERTS, D_FF_EXPERT, D_MODEL), D_FF_EXPERT),
        "g_final": gain(ks[15], (D_MODEL,)),
    }


def reference(x, g_mix, w_in, conv_w, w_branch_conv, w_branch_attn, w_out, g_ffn,
              w_ffn_gate, w_ffn_up, w_ffn_down, w_router, w_exp_gate, w_exp_up,
              w_exp_down, g_final):
    b, s, _ = x.shape
    for i in range(DEPTH):
        h = rmsnorm(x, g_mix[i])
        proj = h @ w_in[i]
        xin, bg, cg, q, k, v, ga, gb = jnp.split(proj, SPLITS, axis=-1)
        ua = bg * causal_dwconv(cg * xin, conv_w[i])
        ub = stick_breaking_attention(q.reshape(b, s, N_HEADS, HEAD_DIM),
                                      k.reshape(b, s, N_HEADS, HEAD_DIM),
                                      v.reshape(b, s, N_HEADS, HEAD_DIM))
        mix = jax.nn.sigmoid(ga) * (ua @ w_branch_conv[i]) + jax.nn.sigmoid(gb) * (ub @ w_branch_attn[i])
        x = x + mix @ w_out[i]
        h = rmsnorm(x, g_ffn[i])
        j = i // 2
        if i % 2 == 0:
            x = x + swiglu(h, w_ffn_gate[j], w_ffn_up[j], w_ffn_down[j])
        else:
            x = x + moe_swiglu(h, w_router[j], w_exp_gate[j], w_exp_up[j], w_exp_down[j])
    return rmsnorm(x, g_final)
```

```python
import numpy as np
from contextlib import ExitStack
import concourse.bass as bass
import concourse.mybir as mybir
from concourse.bass_utils import run_bass_kernel_spmd

F32 = mybir.dt.float32
BF16 = mybir.dt.bfloat16
AF = mybir.ActivationFunctionType
ALU = mybir.AluOpType

COMPUTE = ("pe", "act", "dve", "pool")
ALLENG = ("pe", "act", "dve", "pool", "sp")

S = 4096
D = 1024
NT = 8
DEPTH = 2
DFF = 2816
NE = 8
DFE = 2048
EPS = 1e-6
ARENA_COLS = 52992

C_ID, C_TINC, C_TSTR, C_ONES, C_MASK, C_SEL, C_END = 0, 128, 256, 384, 512, 2560, 3584


class Buf:
    __slots__ = ("name", "w", "r", "dsem")

    def __init__(self, name):
        self.name = name
        self.w = {}
        self.r = {}
        self.dsem = None


class K:
    def __init__(self, nc, es, n_dma_sems=64):
        self.nc = nc
        self.streams = {e: [] for e in ALLENG}
        self.cnt = {e: 0 for e in COMPUTE}
        self.seen = {e: {} for e in ALLENG}
        self.sems = {}
        for e in COMPUTE:
            self.sems[e] = es.enter_context(nc.semaphore("s_" + e))
        self.dtot = {}
        self.qkeys = {"sp": [], "pool": []}
        for i in range(n_dma_sems):
            q = "pool" if i < 16 else "sp"
            key = ("d", q, i)
            self.sems[key] = es.enter_context(nc.semaphore("d%s%d" % (q, i)))
            self.dtot[key] = 0
            self.qkeys[q].append(key)
        self.free = {q: list(v) for q, v in self.qkeys.items()}

    def buf(self, name="b"):
        return Buf(name)

    def bufs(self, n, name="b"):
        return [Buf(name) for _ in range(n)]

    def _emit(self, eng, fn, reads, writes, accw, ev_key, ev_inc):
        need = {}
        for b in reads:
            for kk, v in b.w.items():
                if need.get(kk, 0) < v:
                    need[kk] = v
        for b in writes:
            for kk, v in b.w.items():
                if need.get(kk, 0) < v:
                    need[kk] = v
            for kk, v in b.r.items():
                if need.get(kk, 0) < v:
                    need[kk] = v
        for b in accw:
            for kk, v in b.r.items():
                if need.get(kk, 0) < v:
                    need[kk] = v
        waits = []
        seen = self.seen[eng]
        for kk, v in need.items():
            if kk == "pe" and eng == "pe":
                continue
            if kk in self.dtot:
                v = self.dtot[kk]
            if seen.get(kk, 0) >= v:
                continue
            seen[kk] = v
            waits.append((self.sems[kk], v))
        if ev_key in self.dtot:
            self.dtot[ev_key] += ev_inc
            val = self.dtot[ev_key]
        else:
            self.cnt[ev_key] += 1
            val = self.cnt[ev_key]
        self.streams[eng].append((waits, fn, self.sems[ev_key], ev_inc))
        for b in reads:
            if b.r.get(ev_key, 0) < val:
                b.r[ev_key] = val
        for b in writes:
            b.w = {ev_key: val}
            b.r = {}
        for b in accw:
            b.w[ev_key] = val
            b.r = {}

    def op(self, eng, fn, reads=(), writes=()):
        self._emit(eng, fn, reads, writes, (), eng, 1)

    def dma(self, q, out, in_, sb, reads=(), writes=(), accw=()):
        if sb.dsem is None:
            sb.dsem = {}
        if q not in sb.dsem:
            sb.dsem[q] = self.free[q].pop()
        self._emit(q, lambda e: e.dma_start(out=out, in_=in_), reads, writes, accw, sb.dsem[q], 16)

    def mm(self, out, lhsT, rhs, start, stop, reads, writes):
        self.op("pe", lambda e: e.matmul(out, lhsT, rhs, start=start, stop=stop), reads, writes)

    def tr(self, out, in_, ident, reads, writes):
        self.op("pe", lambda e: e.transpose(out=out, in_=in_, identity=ident), reads, writes)

    def act(self, out, in_, func, reads, writes, scale=None, bias=None):
        kw = {}
        if scale is not None:
            kw["scale"] = scale
        if bias is not None:
            kw["bias"] = bias
        self.op("act", lambda e: e.activation(out=out, in_=in_, func=func, **kw), reads, writes)

    def tt(self, eng, out, in0, in1, op, reads, writes):
        self.op(eng, lambda e: e.tensor_tensor(out=out, in0=in0, in1=in1, op=op), reads, writes)

    def ts(self, eng, out, in0, s1, s2, op0, op1, reads, writes):
        if op1 is None:
            self.op(eng, lambda e: e.tensor_scalar(out=out, in0=in0, scalar1=s1, scalar2=None, op0=op0), reads, writes)
        else:
            self.op(eng, lambda e: e.tensor_scalar(out=out, in0=in0, scalar1=s1, scalar2=s2, op0=op0, op1=op1), reads, writes)

    def stt(self, out, in0, scalar, in1, op0, op1, reads, writes):
        self.op("dve", lambda e: e.scalar_tensor_tensor(out=out, in0=in0, scalar=scalar, in1=in1, op0=op0, op1=op1), reads, writes)

    def copy(self, eng, out, in_, reads, writes):
        if eng == "act":
            self.op("act", lambda e: e.activation(out=out, in_=in_, func=AF.Copy), reads, writes)
        else:
            self.op(eng, lambda e: e.tensor_copy(out=out, in_=in_), reads, writes)

    def barrier(self):
        for e in ALLENG:
            seen = self.seen[e]
            w = []
            for kk in COMPUTE:
                v = self.cnt[kk]
                if kk != e and seen.get(kk, 0) < v:
                    seen[kk] = v
                    w.append((self.sems[kk], v))
            for kk, v in self.dtot.items():
                if v > 0 and seen.get(kk, 0) < v:
                    seen[kk] = v
                    w.append((self.sems[kk], v))
            if w:
                self.streams[e].append((w, None, None, 0))
        self.free = {q: list(v) for q, v in self.qkeys.items()}

    def replay(self):
        nc = self.nc
        streams = self.streams

        def run(engobj, stream):
            for waits, fn, sem, inc in stream:
                for s_, v in waits:
                    engobj.wait_ge(s_, v)
                if fn is not None:
                    fn(engobj).then_inc(sem, inc)

        with nc.Block() as block:
            @block.tensor
            def _(e):
                run(e, streams["pe"])

            @block.scalar
            def _(e):
                run(e, streams["act"])

            @block.vector
            def _(e):
                run(e, streams["dve"])

            @block.gpsimd
            def _(e):
                run(e, streams["pool"])

            @block.sync
            def _(e):
                run(e, streams["sp"])


class Arena:
    def __init__(self, t, ncols):
        self.t = t
        self.n = ncols
        self.off = 0

    def alloc(self, shape, dt):
        n = 1
        for s_ in shape[1:]:
            n *= s_
        nb = n * (4 if dt == F32 else 2)
        cols = (nb + 31) // 32 * 8
        assert self.off + cols <= self.n, "SBUF arena overflow: need %d have %d" % (self.off + cols, self.n)
        v = self.t[:, self.off:self.off + cols]
        self.off += cols
        if dt != F32:
            v = v.bitcast(dt)
        v = v[:, 0:n]
        if len(shape) == 3:
            v = v.rearrange("p (a b) -> p a b", b=shape[2])
        return v


def build(dbg=None, n_layers=DEPTH):
    nc = bass.Bass("TRN2", target_bir_lowering=False)
    dt_in = lambda name, shape: nc.dram_tensor(name, shape, F32, kind="ExternalInput").ap()
    x = dt_in("x", [S, D])
    vecs = dt_in("vecs", [128, 64])
    consts = dt_in("consts", [128, C_END])
    w_in = dt_in("w_in", [DEPTH, D, 5120])
    w_bc = dt_in("w_branch_conv", [DEPTH, 512, D])
    w_ba = dt_in("w_branch_attn", [DEPTH, 512, D])
    w_out = dt_in("w_out", [DEPTH, D, D])
    w_fg = dt_in("w_ffn_gate", [1, D, DFF])
    w_fu = dt_in("w_ffn_up", [1, D, DFF])
    w_fd = dt_in("w_ffn_down", [1, DFF, D])
    w_rt = dt_in("w_router", [1, D, NE])
    w_eg = dt_in("w_exp_gate", [1, NE, D, DFE])
    w_eu = dt_in("w_exp_up", [1, NE, D, DFE])
    w_ed = dt_in("w_exp_down", [1, NE, DFE, D])
    out = nc.dram_tensor("out", [S, D], F32, kind="ExternalOutput").ap()

    skind = "ExternalOutput" if dbg else "Internal"
    xTs = nc.dram_tensor("xTs", [NT, 128, 8, 512], F32, kind=skind).ap()
    qTs = nc.dram_tensor("qTs", [NT, 128, 4, 512], BF16, kind=skind).ap()
    uaTs = nc.dram_tensor("uaTs", [NT, 128, 4, 512], BF16, kind=skind).ap()
    kTs = nc.dram_tensor("kTs", [128, 4, S], BF16, kind=skind).ap()
    vs = nc.dram_tensor("vs", [S, 512], BF16, kind=skind).ap()
    gTs = nc.dram_tensor("gTs", [NT, 16, 128, 512], BF16, kind=skind).ap()

    with ExitStack() as es:
        k = K(nc, es)
        arena_t = es.enter_context(nc.sbuf_tensor("arena", [128, ARENA_COLS], F32))
        ar = Arena(arena_t, ARENA_COLS)
        ppair = [es.enter_context(nc.psum_tensor("pp%d" % i, [128, 1024], F32)) for i in range(4)]
        banks = [ppair[i // 2][:, (i % 2) * 512:(i % 2 + 1) * 512] for i in range(8)]

        vec = ar.alloc([128, 64], F32)
        ident = ar.alloc([128, 128], F32)
        sel = ar.alloc([128, 1024], F32)
        cb16 = ar.alloc([128, C_MASK + 2048], BF16)
        PERS = ar.off
        b_const = k.buf("const")
        k.dma("sp", vec, vecs, b_const, writes=[b_const])
        b_c2 = k.buf("c2")
        k.dma("sp", ident, consts[:, C_ID:C_ID + 128], b_c2, writes=[b_c2])
        b_c3 = k.buf("c3")
        k.dma("sp", sel, consts[:, C_SEL:C_END], b_c3, writes=[b_c3])
        b_c4 = k.buf("c4")
        k.dma("pool", cb16, consts[:, 0:C_MASK + 2048], b_c4, writes=[b_c4])
        CONSTB = [b_const, b_c2, b_c3, b_c4]
        tinc16 = cb16[:, C_TINC:C_TINC + 128]
        tstr16 = cb16[:, C_TSTR:C_TSTR + 128]
        ones16 = cb16[:, C_ONES:C_ONES + 128]
        mask16 = [cb16[:, C_MASK + j * 512:C_MASK + (j + 1) * 512] for j in range(4)]

        b_xTs = k.bufs(NT, "xTs")
        b_qTs = k.buf("qTs")
        b_uaTs = k.buf("uaTs")
        b_kTs = k.buf("kTs")
        b_vs = k.buf("vs")
        b_gTs = k.buf("gTs")
        bankb = k.bufs(8, "bank")

        def rmsnorm_stats(src_chunks, src_bufs, sqs, sq_b, ssbank, ssb, rstd, rstd_b):
            for c in range(8):
                r = c % 2
                k.act(sqs[r], src_chunks[c], AF.Square, [src_bufs[c]], [sq_b[r]])
                k.mm(ssbank, ones16, sqs[r], c == 0, c == 7, [sq_b[r]] + CONSTB, [ssb])
            k.act(rstd, ssbank, AF.Sqrt, [ssb], [rstd_b], scale=1.0 / D, bias=EPS)
            k.op("dve", lambda e: e.reciprocal(out=rstd, in_=rstd), [rstd_b], [rstd_b])

        for l in range(n_layers):
            ar.off = PERS
            win = ar.alloc([128, 8, 5120], BF16)
            xt = [ar.alloc([128, 8, 512], F32) for _ in range(2)]
            ht = [ar.alloc([128, 8, 512], BF16) for _ in range(2)]
            sq8 = [ar.alloc([128, 512], BF16) for _ in range(8)]
            rstd = [ar.alloc([128, 512], F32) for _ in range(2)]
            xin_sb = [ar.alloc([128, 512], F32) for _ in range(2)]
            cacc = [ar.alloc([128, 512], F32) for _ in range(2)]
            u = [ar.alloc([128, 514], F32) for _ in range(4)]
            NSTG = 6
            stg = [ar.alloc([128, 512], BF16) for _ in range(NSTG)]
            xtm = ar.alloc([128, 4, 1024], F32) if l == 0 else None

            b_win = k.bufs(8, "win")
            b_xt = [k.bufs(8, "xt") for _ in range(2)]
            b_xtd = k.bufs(2, "xtd")
            b_ht = [k.bufs(8, "ht") for _ in range(2)]
            b_sq8 = k.bufs(8, "sq8")
            b_rstd = k.bufs(2, "rstd")
            b_xin = k.bufs(2, "xin")
            b_cacc = k.bufs(2, "cacc")
            b_u = k.bufs(4, "u")
            b_stg = k.bufs(NSTG, "stg")
            b_xtm = k.buf("xtm")

            win_src = w_in[l].rearrange("(kc p) f -> p kc f", p=128)
            FG = [(0, 512), (1024, 1536), (512, 1024), (1536, 2560), (2560, 3072), (3072, 4096), (4096, 5120)]
            b_wing = k.bufs(len(FG), "wing")
            for gi, (f0, f1) in enumerate(FG):
                k.dma("pool", win[:, :, f0:f1], win_src[:, :, f0:f1], b_wing[gi], writes=[b_wing[gi]])

            def wbuf(f):
                for gi, (f0, f1) in enumerate(FG):
                    if f0 <= f < f1:
                        return b_wing[gi]
            for c in range(4):
                k.op("pool", (lambda uu: (lambda e: e.memset(uu, 0.0)))(u[c][:, 0:2]), [], [b_u[c]])

            stg_i = [0]
            bank_i = [0]

            def next_stg():
                i = stg_i[0] % NSTG
                stg_i[0] += 1
                return i

            def next_bank():
                i = bank_i[0] % 5
                bank_i[0] += 1
                return i

            x_tiles = x.rearrange("(n b p) d -> n p b d", b=4, p=128)

            def prep(ti):
                j = ti % 2
                if l == 0:
                    k.dma("sp", xtm, x_tiles[ti], b_xtm, writes=[b_xtm])
                    for c in range(8):
                        bk = 6 + c % 2
                        for b in range(4):
                            k.tr(banks[bk][:, b * 128:(b + 1) * 128], xtm[:, b, c * 128:(c + 1) * 128], ident,
                                 [b_xtm] + CONSTB, [bankb[bk]])
                        k.copy("act" if c % 2 == 0 else "dve", xt[j][:, c, :], banks[bk][:, :], [bankb[bk]],
                               [b_xt[j][c], b_xtd[j]] if False else [b_xt[j][c]])
                    k.dma("sp", xTs[ti], xt[j], b_xtd[j], reads=b_xt[j], writes=[b_xTs[ti]])
                else:
                    k.dma("sp", xt[j], xTs[ti], b_xtd[j], reads=[b_xTs[ti]], writes=b_xt[j])

            def prep_sq(ti):
                j = ti % 2
                for c in range(8):
                    k.act(sq8[c], xt[j][:, c, :], AF.Square, [b_xt[j][c]], [b_sq8[c]])

            def prep_b(ti):
                j = ti % 2
                for c in range(8):
                    k.mm(banks[5][:, :], ones16, sq8[c], c == 0, c == 7, [b_sq8[c]] + CONSTB, [bankb[5]])
                k.act(rstd[j], banks[5][:, :], AF.Sqrt, [bankb[5]], [b_rstd[j]], scale=1.0 / D, bias=EPS)
                k.op("dve", (lambda o_: (lambda e: e.reciprocal(out=o_, in_=o_)))(rstd[j]), [b_rstd[j]], [b_rstd[j]])
                for c in range(8):
                    k.stt(ht[j][:, c, :], xt[j][:, c, :], vec[:, l * 8 + c:l * 8 + c + 1], rstd[j], ALU.mult, ALU.mult,
                          [b_xt[j][c], b_rstd[j]] + CONSTB, [b_ht[j][c]])


            prep(0)
            prep_sq(0)
            prep_b(0)
            for ti in range(NT):
                j = ti % 2
                tok = slice(ti * 512, (ti + 1) * 512)

                def proj_fm(fc):
                    bk = next_bank()
                    for kc in range(8):
                        k.mm(banks[bk][:, :], win[:, kc, fc * 128:(fc + 1) * 128], ht[j][:, kc, :], kc == 0, kc == 7,
                             [wbuf(fc * 128), b_ht[j][kc]], [bankb[bk]])
                    return bk

                for c in range(4):
                    r = c % 2
                    bk = proj_fm(c)
                    k.copy("act", xin_sb[r], banks[bk][:, :], [bankb[bk]], [b_xin[r]])
                    bk = proj_fm(8 + c)
                    k.tt("dve", u[c][:, 2:514], banks[bk][:, :], xin_sb[r], ALU.mult, [bankb[bk], b_xin[r]], [b_u[c]])
                    cw = lambda kk: vec[:, 40 + l * 12 + kk * 4 + c:40 + l * 12 + kk * 4 + c + 1]
                    k.ts("dve", cacc[r], u[c][:, 2:514], cw(2), None, ALU.mult, None, [b_u[c]] + CONSTB, [b_cacc[r]])
                    k.stt(cacc[r], u[c][:, 1:513], cw(1), cacc[r], ALU.mult, ALU.add, [b_u[c], b_cacc[r]], [b_cacc[r]])
                    k.stt(cacc[r], u[c][:, 0:512], cw(0), cacc[r], ALU.mult, ALU.add, [b_u[c], b_cacc[r]], [b_cacc[r]])
                    k.copy("pool", u[c][:, 0:2], u[c][:, 512:514], [b_u[c]], [b_u[c]])
                    bk = proj_fm(4 + c)
                    si = next_stg()
                    k.tt("dve", stg[si], banks[bk][:, :], cacc[r], ALU.mult, [bankb[bk], b_cacc[r]], [b_stg[si]])
                    k.dma("sp", uaTs[ti, :, c, :], stg[si], b_stg[si], reads=[b_stg[si]], accw=[b_uaTs])
                if ti + 1 < NT:
                    prep(ti + 1)
                    j = ti % 2
                for c in range(4):
                    bk = proj_fm(12 + c)
                    si = next_stg()
                    k.copy("act", stg[si], banks[bk][:, :], [bankb[bk]], [b_stg[si]])
                    k.dma("sp", qTs[ti, :, c, :], stg[si], b_stg[si], reads=[b_stg[si]], accw=[b_qTs])
                    bk = proj_fm(16 + c)
                    si = next_stg()
                    k.copy("dve", stg[si], banks[bk][:, :], [bankb[bk]], [b_stg[si]])
                    k.dma("sp", kTs[:, c, tok], stg[si], b_stg[si], reads=[b_stg[si]], accw=[b_kTs])
                for b in range(4):
                    bk = next_bank()
                    for kc in range(8):
                        k.mm(banks[bk][:, :], ht[j][:, kc, b * 128:(b + 1) * 128], win[:, kc, 2560:3072], kc == 0, kc == 7,
                             [wbuf(2560), b_ht[j][kc]], [bankb[bk]])
                    si = next_stg()
                    k.copy("dve", stg[si], banks[bk][:, :], [bankb[bk]], [b_stg[si]])
                    t0 = ti * 512 + b * 128
                    k.dma("sp", vs[t0:t0 + 128, :], stg[si], b_stg[si], reads=[b_stg[si]], accw=[b_vs])
                for gc in range(16):
                    if gc == 1 and ti + 1 < NT:
                        prep_sq(ti + 1)
                    if gc == 8 and ti + 1 < NT:
                        prep_b(ti + 1)
                    bk = proj_fm(24 + gc)
                    si = next_stg()
                    k.act(stg[si], banks[bk][:, :], AF.Sigmoid, [bankb[bk]], [b_stg[si]])
                    k.dma("sp", gTs[ti, gc], stg[si], b_stg[si], reads=[b_stg[si]], accw=[b_gTs])
            k.barrier()
            if dbg == ("A", l):
                break

            ar.off = PERS
            kt = ar.alloc([128, 4, S], BF16)
            vv = ar.alloc([128, 32, 512], BF16)
            wa = ar.alloc([128, 4, D], BF16)
            wb = ar.alloc([128, 4, D], BF16)
            wo = ar.alloc([128, 8, D], BF16)
            qq = [ar.alloc([128, 4, 512], BF16) for _ in range(2)]
            ua = ar.alloc([128, 4, 512], BF16)
            gt = ar.alloc([128, 16, 512], BF16)
            xb = ar.alloc([128, 8, 512], F32)
            NE_ = 3
            Eb = [ar.alloc([128, 1024], F32) for _ in range(NE_)]
            spb = [ar.alloc([128, 1024], BF16) for _ in range(NE_)]
            Gb = [ar.alloc([128, 1024], BF16) for _ in range(2)]
            Ab = [ar.alloc([128, 1024], BF16) for _ in range(2)]
            ub2 = [ar.alloc([128, 4, 512], BF16) for _ in range(2)]
            mix = ar.alloc([128, 8, 512], BF16)
            m1 = [ar.alloc([128, 512], F32) for _ in range(2)]
            m2 = [ar.alloc([128, 512], F32) for _ in range(2)]

            b_kt = k.bufs(4, "kt")
            b_vv = k.bufs(4, "vv")
            b_wa, b_wb, b_wo = k.buf("wa"), k.buf("wb"), k.buf("wo")
            b_qq = k.bufs(2, "qq")
            b_ua = k.buf("ua")
            b_gt = k.buf("gt")
            b_xb = k.bufs(8, "xb")
            b_xbd = k.buf("xbd")
            b_E = k.bufs(NE_, "E")
            b_sp = k.bufs(NE_, "sp")
            b_G = k.bufs(2, "G")
            b_A = k.bufs(2, "A")
            b_ub2 = [k.bufs(4, "ub") for _ in range(2)]
            b_mix = k.bufs(8, "mix")
            b_m1 = k.bufs(2, "m1")
            b_m2 = k.bufs(2, "m2")

            vsrc = vs.rearrange("(n p) f -> p n f", p=128)

            def load_kv(g4):
                k.dma("sp", kt[:, :, g4 * 1024:(g4 + 1) * 1024], kTs[:, :, g4 * 1024:(g4 + 1) * 1024], b_kt[g4],
                      reads=[b_kTs], writes=[b_kt[g4]])
                k.dma("sp", vv[:, g4 * 8:(g4 + 1) * 8, :], vsrc[:, g4 * 8:(g4 + 1) * 8, :], b_vv[g4], reads=[b_vs],
                      writes=[b_vv[g4]])

            load_kv(0)
            k.dma("pool", wa, w_bc[l].rearrange("(kc p) f -> p kc f", p=128), b_wa, writes=[b_wa])
            k.dma("pool", wb, w_ba[l].rearrange("(kc p) f -> p kc f", p=128), b_wb, writes=[b_wb])
            k.dma("pool", wo, w_out[l].rearrange("(kc p) f -> p kc f", p=128), b_wo, writes=[b_wo])

            opair_i = 0
            pend_mix = []
            dve_pending = [None]
            need_load = [False]
            for qi in range(NT):
                jq = qi % 2
                k.dma("sp", qq[jq], qTs[qi], b_qq[jq], reads=[b_qTs], writes=[b_qq[jq]])
                ub, b_ub = ub2[jq], b_ub2[jq]

                def load_mix_inputs(qi=qi):
                    k.dma("sp", ua, uaTs[qi], b_ua, reads=[b_uaTs], writes=[b_ua])
                    k.dma("sp", xb, xTs[qi], b_xbd, reads=[b_xTs[qi]], writes=b_xb)
                    k.dma("sp", gt[:, 0:8, :], gTs[qi, 0:8].rearrange("c p f -> p c f"), b_gt, reads=[b_gTs], writes=[b_gt])
                    k.dma("sp", gt[:, 8:16, :], gTs[qi, 8:16].rearrange("c p f -> p c f"), b_gt, reads=[b_gTs], accw=[b_gt])

                if not pend_mix:
                    load_mix_inputs()
                if qi == 0:
                    for g4 in range(1, 4):
                        load_kv(g4)
                nkb = 4 * qi + 4
                steps = []
                for hp in range(4):
                    for n in range(nkb):
                        kb = nkb - 1 - n
                        steps.append(dict(hp=hp, kb=kb, first=(n == 0), last=(kb == 0),
                                          dj=(kb - 4 * qi) if kb >= 4 * qi else -1, ob=4 + (opair_i + hp) % 2))
                N = len(steps)
                for i_, st in enumerate(steps):
                    st["eb"] = i_ % NE_
                    st["gb"] = i_ % 2
                    st["cs"] = 128 * st["dj"] if st["dj"] > 0 else 0
                ZP, XP = ppair[0], ppair[1]
                zb_, xb_ = [bankb[0], bankb[1]], [bankb[2], bankb[3]]

                def V3(T, cs):
                    if cs == 0:
                        return T
                    return T.rearrange("p (h t) -> p h t", h=2)[:, :, cs:512]

                def e_Z(st):
                    hp, kb, cs = st["hp"], st["kb"], st["cs"]
                    for hh in range(2):
                        pb = hh * 64
                        k.mm(ZP[:, hh * 512 + cs:(hh + 1) * 512], kt[pb:pb + 64, hp, kb * 128:(kb + 1) * 128],
                             qq[jq][pb:pb + 64, hp, cs:512], True, True, [b_kt[kb // 8], b_qq[jq]], [zb_[hh]])

                def e_E(st):
                    eb, cs = st["eb"], st["cs"]
                    k.act(V3(Eb[eb], cs), V3(ZP[:, :], cs), AF.Exp, zb_, [b_E[eb]], scale=0.125)
                    if st["dj"] >= 0:
                        for hh in range(2):
                            k.tt("dve", Eb[eb][:, hh * 512 + cs:(hh + 1) * 512], Eb[eb][:, hh * 512 + cs:(hh + 1) * 512],
                                 mask16[st["dj"]][:, cs:512], ALU.mult, [b_E[eb]] + CONSTB, [b_E[eb]])

                def e_sp(st):
                    eb, cs = st["eb"], st["cs"]
                    k.act(V3(spb[eb], cs), V3(Eb[eb], cs), AF.Ln, [b_E[eb]], [b_sp[eb]], bias=1.0)

                def e_Tinc(st):
                    cs = st["cs"]
                    for hh in range(2):
                        k.mm(XP[:, hh * 512 + cs:(hh + 1) * 512], tinc16, spb[st["eb"]][:, hh * 512 + cs:(hh + 1) * 512],
                             st["first"], True, [b_sp[st["eb"]]] + CONSTB, [xb_[hh]])

                def e_G(st):
                    cs = st["cs"]
                    k.act(V3(Gb[st["gb"]], cs), V3(XP[:, :], cs), AF.Exp, xb_, [b_G[st["gb"]]], scale=-1.0)

                def e_Tstr(st):
                    if st["last"]:
                        return
                    cs = st["cs"]
                    for hh in range(2):
                        k.mm(XP[:, hh * 512 + cs:(hh + 1) * 512], tstr16, spb[st["eb"]][:, hh * 512 + cs:(hh + 1) * 512],
                             False, True, [b_sp[st["eb"]]] + CONSTB, [xb_[hh]])

                def e_A(st):
                    cs = st["cs"]
                    k.tt("dve", V3(Ab[st["gb"]], cs), V3(Eb[st["eb"]], cs), V3(Gb[st["gb"]], cs), ALU.mult,
                         [b_E[st["eb"]], b_G[st["gb"]]], [b_A[st["gb"]]])

                def e_AV(st):
                    hp, kb, ob, cs = st["hp"], st["kb"], st["ob"], st["cs"]
                    for hh in range(2):
                        h = 2 * hp + hh
                        k.mm(banks[ob][hh * 64:(hh + 1) * 64, cs:512], vv[:, kb, h * 64:(h + 1) * 64],
                             Ab[st["gb"]][:, hh * 512 + cs:(hh + 1) * 512], st["first"], st["last"],
                             [b_vv[kb // 8], b_A[st["gb"]]], [bankb[ob]])
                    if st["last"]:
                        k.copy("dve", ub[:, hp, :], banks[ob][:, :], [bankb[ob]], [b_ub[hp]])

                mix_stride = max(1, N // 36)
                for i_ in range(-3, N):
                    if 0 <= i_ < N:
                        e_G(steps[i_])
                        e_Tstr(steps[i_])
                    if 0 <= i_ + 1 < N:
                        e_Tinc(steps[i_ + 1])
                    if 0 <= i_ < N:
                        e_A(steps[i_])
                        if dve_pending[0] is not None:
                            dve_pending[0]()
                            dve_pending[0] = None
                        if need_load[0] and not pend_mix:
                            load_mix_inputs()
                            need_load[0] = False
                    if 0 <= i_ + 2 < N:
                        e_E(steps[i_ + 2])
                    if 0 <= i_ + 3 < N:
                        e_Z(steps[i_ + 3])
                    if 0 <= i_ + 2 < N:
                        e_sp(steps[i_ + 2])
                    if 0 <= i_ < N:
                        e_AV(steps[i_])
                        if pend_mix and i_ % mix_stride == 0:
                            pe_, dv_ = pend_mix.pop(0)
                            pe_()
                            dve_pending[0] = dv_
                            if not pend_mix:
                                need_load[0] = True
                opair_i += 4

                if dve_pending[0] is not None:
                    dve_pending[0]()
                    dve_pending[0] = None
                if need_load[0]:
                    load_mix_inputs()
                    need_load[0] = False

                def mk_chunks(qi=qi, ub=ub, b_ub=b_ub):
                    ch = []
                    for oc in range(8):
                        r = oc % 2

                        def peA(oc=oc):
                            for kc in range(4):
                                k.mm(banks[6][:, :], wa[:, kc, oc * 128:(oc + 1) * 128], ua[:, kc, :], kc == 0, kc == 3,
                                     [b_wa, b_ua], [bankb[6]])

                        def dvA(oc=oc, r=r):
                            k.tt("dve", m1[r], banks[6][:, :], gt[:, oc, :], ALU.mult, [bankb[6], b_gt], [b_m1[r]])

                        def peB(oc=oc):
                            for kc in range(4):
                                k.mm(banks[7][:, :], wb[:, kc, oc * 128:(oc + 1) * 128], ub[:, kc, :], kc == 0, kc == 3,
                                     [b_wb, b_ub[kc]], [bankb[7]])

                        def dvB(oc=oc, r=r):
                            k.tt("dve", m2[r], banks[7][:, :], gt[:, 8 + oc, :], ALU.mult, [bankb[7], b_gt], [b_m2[r]])
                            k.tt("pool", mix[:, oc, :], m1[r], m2[r], ALU.add, [b_m1[r], b_m2[r]], [b_mix[oc]])
                        ch += [(peA, dvA), (peB, dvB)]
                    for oc in range(8):
                        bk = 6 + oc % 2

                        def peC(oc=oc, bk=bk):
                            for kc in range(4):
                                k.mm(banks[bk][:, :], wo[:, kc, oc * 128:(oc + 1) * 128], mix[:, kc, :], kc == 0, False,
                                     [b_wo, b_mix[kc]], [bankb[bk]])

                        def peD(oc=oc, bk=bk):
                            for kc in range(4, 8):
                                k.mm(banks[bk][:, :], wo[:, kc, oc * 128:(oc + 1) * 128], mix[:, kc, :], False, kc == 7,
                                     [b_wo, b_mix[kc]], [bankb[bk]])

                        def dvD(oc=oc, bk=bk):
                            k.tt("dve", xb[:, oc, :], xb[:, oc, :], banks[bk][:, :], ALU.add, [bankb[bk], b_xb[oc]], [b_xb[oc]])
                            if oc == 7:
                                k.dma("sp", xTs[qi], xb, b_xbd, reads=b_xb, writes=[b_xTs[qi]])
                        ch += [(peC, None), (peD, dvD)]
                    return ch

                assert not pend_mix
                pend_mix = mk_chunks()
            while pend_mix:
                pe_, dv_ = pend_mix.pop(0)
                pe_()
                if dv_ is not None:
                    dv_()
            k.barrier()
            if dbg == ("B", l):
                break

            moe = (l % 2 == 1)
            last = (l == DEPTH - 1)
            PT = 2048
            NPASS = S // PT
            TPP = PT // 512
            ar.off = PERS
            acc = ar.alloc([128, 8, PT], F32)
            h2 = ar.alloc([128, 8, PT], BF16)
            wg = [ar.alloc([128, 8, 512], BF16) for _ in range(2)]
            wu = [ar.alloc([128, 8, 512], BF16) for _ in range(2)]
            wd = [ar.alloc([128, 4, D], BF16) for _ in range(2)]
            hid = [ar.alloc([128, 4, 512], BF16) for _ in range(2)]
            sil = [ar.alloc([128, 512], F32) for _ in range(2)]
            sil2 = [ar.alloc([128, 512], F32) for _ in range(2)]
            cbb = [ar.alloc([128, 512], F32) for _ in range(4 if moe else 2)]
            h2f = [ar.alloc([128, 512], F32) for _ in range(2)]
            sqs = [ar.alloc([128, 512], BF16) for _ in range(2)]
            rstd = ar.alloc([128, 512], F32)
            combT = ar.alloc([128, PT], F32)
            wr = ar.alloc([128, 8, NE], F32)
            sm = [ar.alloc([128, 64], F32) for _ in range(2)]
            yf = ar.alloc([128, 8, 128], F32) if last else None
            ost = [ar.alloc([128, D], F32) for _ in range(2)] if last else None

            b_acc = k.bufs(8 * TPP, "acc")
            b_accd = k.bufs(TPP, "accd")
            b_h2 = k.bufs(8 * TPP, "h2")
            b_wg, b_wu, b_wd = k.bufs(2, "wg"), k.bufs(2, "wu"), k.bufs(2, "wd")
            b_hid = [k.bufs(4, "hid") for _ in range(2)]
            b_sil, b_sil2, b_cbb = k.bufs(2, "sil"), k.bufs(2, "sil2"), k.bufs(4, "cbb")
            b_h2f = k.bufs(2, "h2f")
            b_sq = k.bufs(2, "sq")
            b_rstd = k.buf("rstd")
            b_combT = k.bufs(TPP, "combT")
            b_wr = k.buf("wr")
            b_sm = k.bufs(2, "sm")
            b_yf = k.bufs(8, "yf")
            b_ost = k.bufs(2, "ost")

            if moe:
                k.dma("sp", wr, w_rt[0].rearrange("(kc p) e -> p kc e", p=128), b_wr, writes=[b_wr])
                units = [(e, off, 4) for e in range(NE) for off in range(0, DFE, 512)]
            else:
                units = [(0, off, 4) for off in range(0, 2560, 512)] + [(0, 2560, 2)]

            def load_unit(ui):
                e, off, nh = units[ui]
                p = ui % 2
                hc = nh * 128
                if moe:
                    gsrc, usrc, dsrc = w_eg[0, e], w_eu[0, e], w_ed[0, e]
                else:
                    gsrc, usrc, dsrc = w_fg[0], w_fu[0], w_fd[0]
                k.dma("pool", wg[p][:, :, 0:hc], gsrc.rearrange("(kc p) f -> p kc f", p=128)[:, :, off:off + hc], b_wg[p],
                      writes=[b_wg[p]])
                k.dma("pool", wu[p][:, :, 0:hc], usrc.rearrange("(kc p) f -> p kc f", p=128)[:, :, off:off + hc], b_wu[p],
                      writes=[b_wu[p]])
                k.dma("pool", wd[p][:, 0:nh, :], dsrc[off:off + hc, :].rearrange("(kc p) f -> p kc f", p=128), b_wd[p],
                      writes=[b_wd[p]])

            gcol = 16 + l * 8
            for ps_ in range(NPASS):
                for tl in range(TPP):
                    ti = ps_ * TPP + tl
                    k.dma("sp", acc[:, :, tl * 512:(tl + 1) * 512], xTs[ti], b_accd[tl], reads=[b_xTs[ti]],
                          writes=b_acc[tl * 8:(tl + 1) * 8])
                load_unit(0)

                def norm_tile(tl):
                    cols = slice(tl * 512, (tl + 1) * 512)
                    ab = b_acc[tl * 8:(tl + 1) * 8]
                    rmsnorm_stats([acc[:, c, cols] for c in range(8)], ab, sqs, b_sq, banks[7][:, :], bankb[7], rstd, b_rstd)
                    for c in range(8):
                        if moe:
                            r = c % 2
                            k.stt(h2f[r], acc[:, c, cols], vec[:, gcol + c:gcol + c + 1], rstd, ALU.mult, ALU.mult,
                                  [ab[c], b_rstd] + CONSTB, [b_h2f[r]])
                            k.copy("act", h2[:, c, cols], h2f[r], [b_h2f[r]], [b_h2[tl * 8 + c]])
                            for b in range(4):
                                k.mm(banks[b][:, 0:8], h2f[r][:, b * 128:(b + 1) * 128], wr[:, c, :], c == 0,
                                     c == 7, [b_h2f[r], b_wr], [bankb[b]])
                        else:
                            k.stt(h2[:, c, cols], acc[:, c, cols], vec[:, gcol + c:gcol + c + 1], rstd, ALU.mult, ALU.mult,
                                  [ab[c], b_rstd] + CONSTB, [b_h2[tl * 8 + c]])
                    if moe:
                        for b in range(4):
                            r = b % 2
                            t_ = sm[r]
                            lg, m8, dd, ee, w1, w2, c1, c2 = (t_[:, 0:8], t_[:, 8:16], t_[:, 16:17], t_[:, 17:18],
                                                              t_[:, 18:19], t_[:, 19:20], t_[:, 24:32], t_[:, 32:40])
                            bs = [b_sm[r]]
                            k.copy("dve", lg, banks[b][:, 0:8], [bankb[b]], bs)
                            k.op("dve", (lambda o_, i_: (lambda e: e.max(out=o_, in_=i_)))(m8, lg), bs, bs)
                            k.tt("dve", dd, m8[:, 1:2], m8[:, 0:1], ALU.subtract, bs, bs)
                            k.act(ee, dd, AF.Exp, bs, bs)
                            k.ts("dve", w1, ee, 1.0, None, ALU.add, None, bs, bs)
                            k.op("dve", (lambda o_: (lambda e: e.reciprocal(out=o_, in_=o_)))(w1), bs, bs)
                            k.tt("dve", w2, ee, w1, ALU.mult, bs, bs)
                            k.ts("dve", c1, lg, m8[:, 0:1], w1, ALU.is_equal, ALU.mult, bs, bs)
                            k.ts("dve", c2, lg, m8[:, 1:2], w2, ALU.is_equal, ALU.mult, bs, bs)
                            k.tt("dve", c1, c1, c2, ALU.add, bs, bs)
                            k.tr(banks[7][0:8, b * 128:(b + 1) * 128], c1, ident, bs + CONSTB, [bankb[7]])
                        k.copy("dve", combT[0:8, cols], banks[7][0:8, :], [bankb[7]], [b_combT[tl]])

                pend = []
                pdi = [0]
                it = 0
                for ui in range(len(units)):
                    e_, off, nh = units[ui]
                    p = ui % 2
                    for tl in range(TPP):
                        cols = slice(tl * 512, (tl + 1) * 512)
                        hp_ = it % 2
                        it += 1
                        if ui == 0:
                            norm_tile(tl)
                        if moe:
                            if off == 0:
                                k.mm(banks[7][:, :], sel[0:8, e_ * 128:(e_ + 1) * 128], combT[0:8, cols], True, True,
                                     [b_combT[tl]] + CONSTB, [bankb[7]])
                                k.copy("act", cbb[tl], banks[7][:, :], [bankb[7]], [b_cbb[tl]])
                        for hc in range(nh):
                            r = hc % 2
                            for kc in range(8):
                                k.mm(banks[r][:, :], wg[p][:, kc, hc * 128:(hc + 1) * 128], h2[:, kc, cols], kc == 0, kc == 7,
                                     [b_wg[p], b_h2[tl * 8 + kc]], [bankb[r]])
                            for kc in range(8):
                                k.mm(banks[2 + r][:, :], wu[p][:, kc, hc * 128:(hc + 1) * 128], h2[:, kc, cols], kc == 0,
                                     kc == 7, [b_wu[p], b_h2[tl * 8 + kc]], [bankb[2 + r]])
                            k.act(sil[r], banks[r][:, :], AF.Silu, [bankb[r]], [b_sil[r]])
                            if moe:
                                k.tt("pool", sil2[r], sil[r], cbb[tl], ALU.mult, [b_sil[r], b_cbb[tl]], [b_sil2[r]])
                                k.tt("dve", hid[hp_][:, hc, :], sil2[r], banks[2 + r][:, :], ALU.mult,
                                     [b_sil2[r], bankb[2 + r]], [b_hid[hp_][hc]])
                            else:
                                k.tt("dve", hid[hp_][:, hc, :], sil[r], banks[2 + r][:, :], ALU.mult,
                                     [b_sil[r], bankb[2 + r]], [b_hid[hp_][hc]])
                            for _ in range(8 // nh):
                                if pend:
                                    pend.pop(0)()

                        def mk_down(oc, p=p, nh=nh, hp_=hp_, tl=tl, cols=cols):
                            def f_():
                                bk = 4 + pdi[0] % 3
                                pdi[0] += 1
                                for hc in range(nh):
                                    k.mm(banks[bk][:, :], wd[p][:, hc, oc * 128:(oc + 1) * 128], hid[hp_][:, hc, :], hc == 0,
                                         hc == nh - 1, [b_wd[p], b_hid[hp_][hc]], [bankb[bk]])
                                k.tt("dve", acc[:, oc, cols], acc[:, oc, cols], banks[bk][:, :], ALU.add,
                                     [bankb[bk], b_acc[tl * 8 + oc]], [b_acc[tl * 8 + oc]])
                            return f_
                        while pend:
                            pend.pop(0)()
                        pend.extend(mk_down(oc) for oc in range(8))
                        if tl == 0 and ui + 1 < len(units):
                            load_unit(ui + 1)
                while pend:
                    pend.pop(0)()

                for tl in range(TPP):
                    ti = ps_ * TPP + tl
                    cols = slice(tl * 512, (tl + 1) * 512)
                    ab = b_acc[tl * 8:(tl + 1) * 8]
                    if not last:
                        k.dma("sp", xTs[ti], acc[:, :, cols], b_accd[tl], reads=ab, writes=[b_xTs[ti]])
                    else:
                        rmsnorm_stats([acc[:, c, cols] for c in range(8)], ab, sqs, b_sq, banks[7][:, :], bankb[7], rstd,
                                      b_rstd)
                        for b in range(4):
                            r = b % 2
                            bc = slice(tl * 512 + b * 128, tl * 512 + (b + 1) * 128)
                            for c in range(8):
                                k.stt(yf[:, c, :], acc[:, c, bc], vec[:, 32 + c:33 + c], rstd[:, b * 128:(b + 1) * 128],
                                      ALU.mult, ALU.mult, [ab[c], b_rstd] + CONSTB, [b_yf[c]])
                            for c in range(8):
                                bk = 5 + c // 4
                                k.tr(banks[bk][:, (c % 4) * 128:(c % 4 + 1) * 128], yf[:, c, :], ident,
                                     [b_yf[c]] + CONSTB, [bankb[bk]])
                            k.copy("act", ost[r][:, 0:512], banks[5][:, :], [bankb[5]], [b_ost[r]])
                            k.copy("dve", ost[r][:, 512:1024], banks[6][:, :], [bankb[6]], [b_ost[r]])
                            t0 = ti * 512 + b * 128
                            k.dma("sp", out[t0:t0 + 128, :], ost[r], b_ost[r], reads=[b_ost[r]])
            k.barrier()
            if dbg == ("C", l):
                break

        k.barrier()
        k.replay()
    return nc


def _consts():
    c = np.zeros((128, C_END), np.float32)
    c[:, C_ID:C_ID + 128] = np.eye(128, dtype=np.float32)
    j = np.arange(128)[:, None]
    s_ = np.arange(128)[None, :]
    c[:, C_TINC:C_TINC + 128] = (j >= s_)
    c[:, C_TSTR:C_TSTR + 128] = (j < s_)
    c[:, C_ONES:C_ONES + 128] = 1.0
    t = np.arange(512)[None, :]
    for jj in range(4):
        c[:, C_MASK + jj * 512:C_MASK + (jj + 1) * 512] = (t > 128 * jj + j)
    for e in range(NE):
        c[e, C_SEL + e * 128:C_SEL + (e + 1) * 128] = 1.0
    return c


def _vecs(g_mix, g_ffn, g_final, conv_w):
    v = np.zeros((128, 64), np.float32)
    for l in range(DEPTH):
        v[:, l * 8:(l + 1) * 8] = np.asarray(g_mix[l]).reshape(8, 128).T
        v[:, 16 + l * 8:16 + (l + 1) * 8] = np.asarray(g_ffn[l]).reshape(8, 128).T
        for kk in range(3):
            v[:, 40 + l * 12 + kk * 4:40 + l * 12 + kk * 4 + 4] = np.asarray(conv_w[l, kk]).reshape(4, 128).T
    v[:, 32:40] = np.asarray(g_final).reshape(8, 128).T
    return v


_NC_CACHE = {}


def kernel(x, g_mix, w_in, conv_w, w_branch_conv, w_branch_attn, w_out, g_ffn, w_ffn_gate, w_ffn_up, w_ffn_down,
           w_router, w_exp_gate, w_exp_up, w_exp_down, g_final):
    f = lambda a: np.ascontiguousarray(np.asarray(a, dtype=np.float32))
    x = f(x)
    shared = {
        "vecs": _vecs(f(g_mix), f(g_ffn), f(g_final), f(conv_w)),
        "consts": _consts(),
        "w_in": f(w_in), "w_branch_conv": f(w_branch_conv), "w_branch_attn": f(w_branch_attn), "w_out": f(w_out),
        "w_ffn_gate": f(w_ffn_gate), "w_ffn_up": f(w_ffn_up), "w_ffn_down": f(w_ffn_down), "w_router": f(w_router),
        "w_exp_gate": f(w_exp_gate), "w_exp_up": f(w_exp_up), "w_exp_down": f(w_exp_down),
    }
    if "nc" not in _NC_CACHE:
        _NC_CACHE["nc"] = build()
    nc = _NC_CACHE["nc"]
    in_maps = []
    for i in range(8):
        m = dict(shared)
        m["x"] = x[i]
        in_maps.append(m)
    res = run_bass_kernel_spmd(nc, in_maps, core_ids=list(range(8)))
    return np.stack([r["out"] for r in res.results], axis=0).astype(np.float32)
```

```python
import numpy as np
from contextlib import ExitStack
import concourse.bass as bass
import concourse.mybir as mybir
from concourse.bass_utils import run_bass_kernel_spmd

F32 = mybir.dt.float32
BF16 = mybir.dt.bfloat16
AF = mybir.ActivationFunctionType
ALU = mybir.AluOpType

COMPUTE = ("pe", "act", "dve", "pool")
ALLENG = ("pe", "act", "dve", "pool", "sp")

S = 4096
D = 1024
NT = 8
DEPTH = 2
DFF = 2816
NE = 8
DFE = 2048
EPS = 1e-6
ARENA_COLS = 52992

C_ID, C_TINC, C_TSTR, C_ONES, C_MASK, C_SEL, C_END = 0, 128, 256, 384, 512, 2560, 3584


class Buf:
    __slots__ = ("name", "w", "r", "dsem")

    def __init__(self, name):
        self.name = name
        self.w = {}
        self.r = {}
        self.dsem = None


class K:
    def __init__(self, nc, es, n_dma_sems=64):
        self.nc = nc
        self.streams = {e: [] for e in ALLENG}
        self.cnt = {e: 0 for e in COMPUTE}
        self.seen = {e: {} for e in ALLENG}
        self.sems = {}
        for e in COMPUTE:
            self.sems[e] = es.enter_context(nc.semaphore("s_" + e))
        self.dtot = {}
        self.qkeys = {"sp": [], "pool": []}
        for i in range(n_dma_sems):
            q = "pool" if i < 16 else "sp"
            key = ("d", q, i)
            self.sems[key] = es.enter_context(nc.semaphore("d%s%d" % (q, i)))
            self.dtot[key] = 0
            self.qkeys[q].append(key)
        self.free = {q: list(v) for q, v in self.qkeys.items()}

    def buf(self, name="b"):
        return Buf(name)

    def bufs(self, n, name="b"):
        return [Buf(name) for _ in range(n)]

    def _emit(self, eng, fn, reads, writes, accw, ev_key, ev_inc):
        need = {}
        for b in reads:
            for kk, v in b.w.items():
                if need.get(kk, 0) < v:
                    need[kk] = v
        for b in writes:
            for kk, v in b.w.items():
                if need.get(kk, 0) < v:
                    need[kk] = v
            for kk, v in b.r.items():
                if need.get(kk, 0) < v:
                    need[kk] = v
        for b in accw:
            for kk, v in b.r.items():
                if need.get(kk, 0) < v:
                    need[kk] = v
        waits = []
        seen = self.seen[eng]
        for kk, v in need.items():
            if kk == "pe" and eng == "pe":
                continue
            if kk in self.dtot:
                v = self.dtot[kk]
            if seen.get(kk, 0) >= v:
                continue
            seen[kk] = v
            waits.append((self.sems[kk], v))
        if ev_key in self.dtot:
            self.dtot[ev_key] += ev_inc
            val = self.dtot[ev_key]
        else:
            self.cnt[ev_key] += 1
            val = self.cnt[ev_key]
        self.streams[eng].append((waits, fn, self.sems[ev_key], ev_inc))
        for b in reads:
            if b.r.get(ev_key, 0) < val:
                b.r[ev_key] = val
        for b in writes:
            b.w = {ev_key: val}
            b.r = {}
        for b in accw:
            b.w[ev_key] = val
            b.r = {}

    def op(self, eng, fn, reads=(), writes=()):
        self._emit(eng, fn, reads, writes, (), eng, 1)

    def dma(self, q, out, in_, sb, reads=(), writes=(), accw=()):
        if sb.dsem is None:
            sb.dsem = {}
        if q not in sb.dsem:
            sb.dsem[q] = self.free[q].pop()
        self._emit(q, lambda e: e.dma_start(out=out, in_=in_), reads, writes, accw, sb.dsem[q], 16)

    def mm(self, out, lhsT, rhs, start, stop, reads, writes):
        self.op("pe", lambda e: e.matmul(out, lhsT, rhs, start=start, stop=stop), reads, writes)

    def tr(self, out, in_, ident, reads, writes):
        self.op("pe", lambda e: e.transpose(out=out, in_=in_, identity=ident), reads, writes)

    def act(self, out, in_, func, reads, writes, scale=None, bias=None):
        kw = {}
        if scale is not None:
            kw["scale"] = scale
        if bias is not None:
            kw["bias"] = bias
        self.op("act", lambda e: e.activation(out=out, in_=in_, func=func, **kw), reads, writes)

    def tt(self, eng, out, in0, in1, op, reads, writes):
        self.op(eng, lambda e: e.tensor_tensor(out=out, in0=in0, in1=in1, op=op), reads, writes)

    def ts(self, eng, out, in0, s1, s2, op0, op1, reads, writes):
        if op1 is None:
            self.op(eng, lambda e: e.tensor_scalar(out=out, in0=in0, scalar1=s1, scalar2=None, op0=op0), reads, writes)
        else:
            self.op(eng, lambda e: e.tensor_scalar(out=out, in0=in0, scalar1=s1, scalar2=s2, op0=op0, op1=op1), reads, writes)

    def stt(self, out, in0, scalar, in1, op0, op1, reads, writes):
        self.op("dve", lambda e: e.scalar_tensor_tensor(out=out, in0=in0, scalar=scalar, in1=in1, op0=op0, op1=op1), reads, writes)

    def copy(self, eng, out, in_, reads, writes):
        if eng == "act":
            self.op("act", lambda e: e.activation(out=out, in_=in_, func=AF.Copy), reads, writes)
        else:
            self.op(eng, lambda e: e.tensor_copy(out=out, in_=in_), reads, writes)

    def barrier(self):
        for e in ALLENG:
            seen = self.seen[e]
            w = []
            for kk in COMPUTE:
                v = self.cnt[kk]
                if kk != e and seen.get(kk, 0) < v:
                    seen[kk] = v
                    w.append((self.sems[kk], v))
            for kk, v in self.dtot.items():
                if v > 0 and seen.get(kk, 0) < v:
                    seen[kk] = v
                    w.append((self.sems[kk], v))
            if w:
                self.streams[e].append((w, None, None, 0))
        self.free = {q: list(v) for q, v in self.qkeys.items()}

    def replay(self):
        nc = self.nc
        streams = self.streams

        def run(engobj, stream):
            for waits, fn, sem, inc in stream:
                for s_, v in waits:
                    engobj.wait_ge(s_, v)
                if fn is not None:
                    fn(engobj).then_inc(sem, inc)

        with nc.Block() as block:
            @block.tensor
            def _(e):
                run(e, streams["pe"])

            @block.scalar
            def _(e):
                run(e, streams["act"])

            @block.vector
            def _(e):
                run(e, streams["dve"])

            @block.gpsimd
            def _(e):
                run(e, streams["pool"])

            @block.sync
            def _(e):
                run(e, streams["sp"])


class Arena:
    def __init__(self, t, ncols):
        self.t = t
        self.n = ncols
        self.off = 0

    def alloc(self, shape, dt):
        n = 1
        for s_ in shape[1:]:
            n *= s_
        nb = n * (4 if dt == F32 else 2)
        cols = (nb + 31) // 32 * 8
        assert self.off + cols <= self.n, "SBUF arena overflow: need %d have %d" % (self.off + cols, self.n)
        v = self.t[:, self.off:self.off + cols]
        self.off += cols
        if dt != F32:
            v = v.bitcast(dt)
        v = v[:, 0:n]
        if len(shape) == 3:
            v = v.rearrange("p (a b) -> p a b", b=shape[2])
        return v


def build(dbg=None, n_layers=DEPTH):
    nc = bass.Bass("TRN2", target_bir_lowering=False)
    dt_in = lambda name, shape: nc.dram_tensor(name, shape, F32, kind="ExternalInput").ap()
    x = dt_in("x", [S, D])
    vecs = dt_in("vecs", [128, 64])
    consts = dt_in("consts", [128, C_END])
    w_in = dt_in("w_in", [DEPTH, D, 5120])
    w_bc = dt_in("w_branch_conv", [DEPTH, 512, D])
    w_ba = dt_in("w_branch_attn", [DEPTH, 512, D])
    w_out = dt_in("w_out", [DEPTH, D, D])
    w_fg = dt_in("w_ffn_gate", [1, D, DFF])
    w_fu = dt_in("w_ffn_up", [1, D, DFF])
    w_fd = dt_in("w_ffn_down", [1, DFF, D])
    w_rt = dt_in("w_router", [1, D, NE])
    w_eg = dt_in("w_exp_gate", [1, NE, D, DFE])
    w_eu = dt_in("w_exp_up", [1, NE, D, DFE])
    w_ed = dt_in("w_exp_down", [1, NE, DFE, D])
    out = nc.dram_tensor("out", [S, D], F32, kind="ExternalOutput").ap()

    skind = "ExternalOutput" if dbg else "Internal"
    xTs = nc.dram_tensor("xTs", [NT, 128, 8, 512], F32, kind=skind).ap()
    qTs = nc.dram_tensor("qTs", [NT, 128, 4, 512], BF16, kind=skind).ap()
    uaTs = nc.dram_tensor("uaTs", [NT, 128, 4, 512], BF16, kind=skind).ap()
    kTs = nc.dram_tensor("kTs", [128, 4, S], BF16, kind=skind).ap()
    vs = nc.dram_tensor("vs", [S, 512], BF16, kind=skind).ap()
    gTs = nc.dram_tensor("gTs", [NT, 16, 128, 512], BF16, kind=skind).ap()

    with ExitStack() as es:
        k = K(nc, es)
        arena_t = es.enter_context(nc.sbuf_tensor("arena", [128, ARENA_COLS], F32))
        ar = Arena(arena_t, ARENA_COLS)
        ppair = [es.enter_context(nc.psum_tensor("pp%d" % i, [128, 1024], F32)) for i in range(4)]
        banks = [ppair[i // 2][:, (i % 2) * 512:(i % 2 + 1) * 512] for i in range(8)]

        vec = ar.alloc([128, 64], F32)
        ident = ar.alloc([128, 128], F32)
        sel = ar.alloc([128, 1024], F32)
        cb16 = ar.alloc([128, C_MASK + 2048], BF16)
        PERS = ar.off
        b_const = k.buf("const")
        k.dma("sp", vec, vecs, b_const, writes=[b_const])
        b_c2 = k.buf("c2")
        k.dma("sp", ident, consts[:, C_ID:C_ID + 128], b_c2, writes=[b_c2])
        b_c3 = k.buf("c3")
        k.dma("sp", sel, consts[:, C_SEL:C_END], b_c3, writes=[b_c3])
        b_c4 = k.buf("c4")
        k.dma("pool", cb16, consts[:, 0:C_MASK + 2048], b_c4, writes=[b_c4])
        CONSTB = [b_const, b_c2, b_c3, b_c4]
        tinc16 = cb16[:, C_TINC:C_TINC + 128]
        tstr16 = cb16[:, C_TSTR:C_TSTR + 128]
        ones16 = cb16[:, C_ONES:C_ONES + 128]
        mask16 = [cb16[:, C_MASK + j * 512:C_MASK + (j + 1) * 512] for j in range(4)]

        b_xTs = k.bufs(NT, "xTs")
        b_qTs = k.buf("qTs")
        b_uaTs = k.buf("uaTs")
        b_kTs = k.buf("kTs")
        b_vs = k.buf("vs")
        b_gTs = k.buf("gTs")
        bankb = k.bufs(8, "bank")

        def rmsnorm_stats(src_chunks, src_bufs, sqs, sq_b, ssbank, ssb, rstd, rstd_b):
            for c in range(8):
                r = c % 2
                k.act(sqs[r], src_chunks[c], AF.Square, [src_bufs[c]], [sq_b[r]])
                k.mm(ssbank, ones16, sqs[r], c == 0, c == 7, [sq_b[r]] + CONSTB, [ssb])
            k.act(rstd, ssbank, AF.Sqrt, [ssb], [rstd_b], scale=1.0 / D, bias=EPS)
            k.op("dve", lambda e: e.reciprocal(out=rstd, in_=rstd), [rstd_b], [rstd_b])

        for l in range(n_layers):
            ar.off = PERS
            win = ar.alloc([128, 8, 5120], BF16)
            xt = [ar.alloc([128, 8, 512], F32) for _ in range(2)]
            ht = [ar.alloc([128, 8, 512], BF16) for _ in range(2)]
            sq8 = [ar.alloc([128, 512], BF16) for _ in range(8)]
            rstd = [ar.alloc([128, 512], F32) for _ in range(2)]
            xin_sb = [ar.alloc([128, 512], F32) for _ in range(2)]
            cacc = [ar.alloc([128, 512], F32) for _ in range(2)]
            u = [ar.alloc([128, 514], F32) for _ in range(4)]
            NSTG = 6
            stg = [ar.alloc([128, 512], BF16) for _ in range(NSTG)]
            xtm = ar.alloc([128, 4, 1024], F32) if l == 0 else None

            b_win = k.bufs(8, "win")
            b_xt = [k.bufs(8, "xt") for _ in range(2)]
            b_xtd = k.bufs(2, "xtd")
            b_ht = [k.bufs(8, "ht") for _ in range(2)]
            b_sq8 = k.bufs(8, "sq8")
            b_rstd = k.bufs(2, "rstd")
            b_xin = k.bufs(2, "xin")
            b_cacc = k.bufs(2, "cacc")
            b_u = k.bufs(4, "u")
            b_stg = k.bufs(NSTG, "stg")
            b_xtm = k.buf("xtm")

            win_src = w_in[l].rearrange("(kc p) f -> p kc f", p=128)
            FG = [(0, 1536), (1536, 2560), (2560, 3072), (3072, 4096), (4096, 5120)]
            b_wing = k.bufs(len(FG), "wing")
            for gi, (f0, f1) in enumerate(FG):
                k.dma("pool", win[:, :, f0:f1], win_src[:, :, f0:f1], b_wing[gi], writes=[b_wing[gi]])

            def wbuf(f):
                for gi, (f0, f1) in enumerate(FG):
                    if f0 <= f < f1:
                        return b_wing[gi]
            for c in range(4):
                k.op("pool", (lambda uu: (lambda e: e.memset(uu, 0.0)))(u[c][:, 0:2]), [], [b_u[c]])

            stg_i = [0]
            bank_i = [0]

            def next_stg():
                i = stg_i[0] % NSTG
                stg_i[0] += 1
                return i

            def next_bank():
                i = bank_i[0] % 5
                bank_i[0] += 1
                return i

            x_tiles = x.rearrange("(n b p) d -> n p b d", b=4, p=128)

            def prep(ti):
                j = ti % 2
                if l == 0:
                    k.dma("sp", xtm, x_tiles[ti], b_xtm, writes=[b_xtm])
                    for c in range(8):
                        bk = 6 + c % 2
                        for b in range(4):
                            k.tr(banks[bk][:, b * 128:(b + 1) * 128], xtm[:, b, c * 128:(c + 1) * 128], ident,
                                 [b_xtm] + CONSTB, [bankb[bk]])
                        k.copy("act" if c % 2 == 0 else "dve", xt[j][:, c, :], banks[bk][:, :], [bankb[bk]],
                               [b_xt[j][c], b_xtd[j]] if False else [b_xt[j][c]])
                    k.dma("sp", xTs[ti], xt[j], b_xtd[j], reads=b_xt[j], writes=[b_xTs[ti]])
                else:
                    k.dma("sp", xt[j], xTs[ti], b_xtd[j], reads=[b_xTs[ti]], writes=b_xt[j])

            def prep_sq(ti):
                j = ti % 2
                for c in range(8):
                    k.act(sq8[c], xt[j][:, c, :], AF.Square, [b_xt[j][c]], [b_sq8[c]])

            def prep_b(ti):
                j = ti % 2
                for c in range(8):
                    k.mm(banks[5][:, :], ones16, sq8[c], c == 0, c == 7, [b_sq8[c]] + CONSTB, [bankb[5]])
                k.act(rstd[j], banks[5][:, :], AF.Sqrt, [bankb[5]], [b_rstd[j]], scale=1.0 / D, bias=EPS)
                k.op("dve", (lambda o_: (lambda e: e.reciprocal(out=o_, in_=o_)))(rstd[j]), [b_rstd[j]], [b_rstd[j]])
                for c in range(8):
                    k.stt(ht[j][:, c, :], xt[j][:, c, :], vec[:, l * 8 + c:l * 8 + c + 1], rstd[j], ALU.mult, ALU.mult,
                          [b_xt[j][c], b_rstd[j]] + CONSTB, [b_ht[j][c]])


            prep(0)
            prep_sq(0)
            prep_b(0)
            for ti in range(NT):
                j = ti % 2
                tok = slice(ti * 512, (ti + 1) * 512)

                def proj_fm(fc):
                    bk = next_bank()
                    for kc in range(8):
                        k.mm(banks[bk][:, :], win[:, kc, fc * 128:(fc + 1) * 128], ht[j][:, kc, :], kc == 0, kc == 7,
                             [wbuf(fc * 128), b_ht[j][kc]], [bankb[bk]])
                    return bk

                for c in range(4):
                    r = c % 2
                    bk = proj_fm(c)
                    k.copy("act", xin_sb[r], banks[bk][:, :], [bankb[bk]], [b_xin[r]])
                    bk = proj_fm(8 + c)
                    k.tt("dve", u[c][:, 2:514], banks[bk][:, :], xin_sb[r], ALU.mult, [bankb[bk], b_xin[r]], [b_u[c]])
                    cw = lambda kk: vec[:, 40 + l * 12 + kk * 4 + c:40 + l * 12 + kk * 4 + c + 1]
                    k.ts("dve", cacc[r], u[c][:, 2:514], cw(2), None, ALU.mult, None, [b_u[c]] + CONSTB, [b_cacc[r]])
                    k.stt(cacc[r], u[c][:, 1:513], cw(1), cacc[r], ALU.mult, ALU.add, [b_u[c], b_cacc[r]], [b_cacc[r]])
                    k.stt(cacc[r], u[c][:, 0:512], cw(0), cacc[r], ALU.mult, ALU.add, [b_u[c], b_cacc[r]], [b_cacc[r]])
                    k.copy("pool", u[c][:, 0:2], u[c][:, 512:514], [b_u[c]], [b_u[c]])
                    bk = proj_fm(4 + c)
                    si = next_stg()
                    k.tt("dve", stg[si], banks[bk][:, :], cacc[r], ALU.mult, [bankb[bk], b_cacc[r]], [b_stg[si]])
                    k.dma("sp", uaTs[ti, :, c, :], stg[si], b_stg[si], reads=[b_stg[si]], accw=[b_uaTs])
                if ti + 1 < NT:
                    prep(ti + 1)
                    j = ti % 2
                for c in range(4):
                    bk = proj_fm(12 + c)
                    si = next_stg()
                    k.copy("act", stg[si], banks[bk][:, :], [bankb[bk]], [b_stg[si]])
                    k.dma("sp", qTs[ti, :, c, :], stg[si], b_stg[si], reads=[b_stg[si]], accw=[b_qTs])
                    bk = proj_fm(16 + c)
                    si = next_stg()
                    k.copy("dve", stg[si], banks[bk][:, :], [bankb[bk]], [b_stg[si]])
                    k.dma("sp", kTs[:, c, tok], stg[si], b_stg[si], reads=[b_stg[si]], accw=[b_kTs])
                for b in range(4):
                    bk = next_bank()
                    for kc in range(8):
                        k.mm(banks[bk][:, :], ht[j][:, kc, b * 128:(b + 1) * 128], win[:, kc, 2560:3072], kc == 0, kc == 7,
                             [wbuf(2560), b_ht[j][kc]], [bankb[bk]])
                    si = next_stg()
                    k.copy("dve", stg[si], banks[bk][:, :], [bankb[bk]], [b_stg[si]])
                    t0 = ti * 512 + b * 128
                    k.dma("sp", vs[t0:t0 + 128, :], stg[si], b_stg[si], reads=[b_stg[si]], accw=[b_vs])
                for gc in range(16):
                    if gc == 1 and ti + 1 < NT:
                        prep_sq(ti + 1)
                    if gc == 8 and ti + 1 < NT:
                        prep_b(ti + 1)
                    bk = proj_fm(24 + gc)
                    si = next_stg()
                    k.act(stg[si], banks[bk][:, :], AF.Sigmoid, [bankb[bk]], [b_stg[si]])
                    k.dma("sp", gTs[ti, gc], stg[si], b_stg[si], reads=[b_stg[si]], accw=[b_gTs])
            k.barrier()
            if dbg == ("A", l):
                break

            ar.off = PERS
            kt = ar.alloc([128, 4, S], BF16)
            vv = ar.alloc([128, 32, 512], BF16)
            wa = ar.alloc([128, 4, D], BF16)
            wb = ar.alloc([128, 4, D], BF16)
            wo = ar.alloc([128, 8, D], BF16)
            qq = [ar.alloc([128, 4, 512], BF16) for _ in range(2)]
            ua = ar.alloc([128, 4, 512], BF16)
            gt = ar.alloc([128, 16, 512], BF16)
            xb = ar.alloc([128, 8, 512], F32)
            NE_ = 3
            Eb = [ar.alloc([128, 1024], F32) for _ in range(NE_)]
            spb = [ar.alloc([128, 1024], BF16) for _ in range(NE_)]
            Gb = [ar.alloc([128, 1024], BF16) for _ in range(2)]
            Ab = [ar.alloc([128, 1024], BF16) for _ in range(2)]
            ub2 = [ar.alloc([128, 4, 512], BF16) for _ in range(2)]
            mix = ar.alloc([128, 8, 512], BF16)
            m1 = [ar.alloc([128, 512], F32) for _ in range(2)]
            m2 = [ar.alloc([128, 512], F32) for _ in range(2)]

            b_kt = k.bufs(4, "kt")
            b_vv = k.bufs(4, "vv")
            b_wa, b_wb, b_wo = k.buf("wa"), k.buf("wb"), k.buf("wo")
            b_qq = k.bufs(2, "qq")
            b_ua = k.buf("ua")
            b_gt = k.buf("gt")
            b_xb = k.bufs(8, "xb")
            b_xbd = k.buf("xbd")
            b_E = k.bufs(NE_, "E")
            b_sp = k.bufs(NE_, "sp")
            b_G = k.bufs(2, "G")
            b_A = k.bufs(2, "A")
            b_ub2 = [k.bufs(4, "ub") for _ in range(2)]
            b_mix = k.bufs(8, "mix")
            b_m1 = k.bufs(2, "m1")
            b_m2 = k.bufs(2, "m2")

            vsrc = vs.rearrange("(n p) f -> p n f", p=128)

            def load_kv(g4):
                k.dma("sp", kt[:, :, g4 * 1024:(g4 + 1) * 1024], kTs[:, :, g4 * 1024:(g4 + 1) * 1024], b_kt[g4],
                      reads=[b_kTs], writes=[b_kt[g4]])
                k.dma("sp", vv[:, g4 * 8:(g4 + 1) * 8, :], vsrc[:, g4 * 8:(g4 + 1) * 8, :], b_vv[g4], reads=[b_vs],
                      writes=[b_vv[g4]])

            load_kv(0)
            k.dma("pool", wa, w_bc[l].rearrange("(kc p) f -> p kc f", p=128), b_wa, writes=[b_wa])
            k.dma("pool", wb, w_ba[l].rearrange("(kc p) f -> p kc f", p=128), b_wb, writes=[b_wb])
            k.dma("pool", wo, w_out[l].rearrange("(kc p) f -> p kc f", p=128), b_wo, writes=[b_wo])

            opair_i = 0
            pend_mix = []
            dve_pending = [None]
            need_load = [False]
            for qi in range(NT):
                jq = qi % 2
                k.dma("sp", qq[jq], qTs[qi], b_qq[jq], reads=[b_qTs], writes=[b_qq[jq]])
                ub, b_ub = ub2[jq], b_ub2[jq]

                def load_mix_inputs(qi=qi):
                    k.dma("sp", ua, uaTs[qi], b_ua, reads=[b_uaTs], writes=[b_ua])
                    k.dma("sp", xb, xTs[qi], b_xbd, reads=[b_xTs[qi]], writes=b_xb)
                    k.dma("sp", gt[:, 0:8, :], gTs[qi, 0:8].rearrange("c p f -> p c f"), b_gt, reads=[b_gTs], writes=[b_gt])
                    k.dma("sp", gt[:, 8:16, :], gTs[qi, 8:16].rearrange("c p f -> p c f"), b_gt, reads=[b_gTs], accw=[b_gt])

                if not pend_mix:
                    load_mix_inputs()
                if qi == 0:
                    for g4 in range(1, 4):
                        load_kv(g4)
                nkb = 4 * qi + 4
                steps = []
                for hp in range(4):
                    for n in range(nkb):
                        kb = nkb - 1 - n
                        steps.append(dict(hp=hp, kb=kb, first=(n == 0), last=(kb == 0),
                                          dj=(kb - 4 * qi) if kb >= 4 * qi else -1, ob=4 + (opair_i + hp) % 2))
                N = len(steps)
                for i_, st in enumerate(steps):
                    st["eb"] = i_ % NE_
                    st["gb"] = i_ % 2
                    st["cs"] = 128 * st["dj"] if st["dj"] > 0 else 0
                ZP, XP = ppair[0], ppair[1]
                zb_, xb_ = [bankb[0], bankb[1]], [bankb[2], bankb[3]]

                def V3(T, cs):
                    if cs == 0:
                        return T
                    return T.rearrange("p (h t) -> p h t", h=2)[:, :, cs:512]

                def e_Z(st):
                    hp, kb, cs = st["hp"], st["kb"], st["cs"]
                    for hh in range(2):
                        pb = hh * 64
                        k.mm(ZP[:, hh * 512 + cs:(hh + 1) * 512], kt[pb:pb + 64, hp, kb * 128:(kb + 1) * 128],
                             qq[jq][pb:pb + 64, hp, cs:512], True, True, [b_kt[kb // 8], b_qq[jq]], [zb_[hh]])

                def e_E(st):
                    eb, cs = st["eb"], st["cs"]
                    k.act(V3(Eb[eb], cs), V3(ZP[:, :], cs), AF.Exp, zb_, [b_E[eb]], scale=0.125)
                    if st["dj"] >= 0:
                        for hh in range(2):
                            k.tt("dve", Eb[eb][:, hh * 512 + cs:(hh + 1) * 512], Eb[eb][:, hh * 512 + cs:(hh + 1) * 512],
                                 mask16[st["dj"]][:, cs:512], ALU.mult, [b_E[eb]] + CONSTB, [b_E[eb]])

                def e_sp(st):
                    eb, cs = st["eb"], st["cs"]
                    k.act(V3(spb[eb], cs), V3(Eb[eb], cs), AF.Ln, [b_E[eb]], [b_sp[eb]], bias=1.0)

                def e_Tinc(st):
                    cs = st["cs"]
                    for hh in range(2):
                        k.mm(XP[:, hh * 512 + cs:(hh + 1) * 512], tinc16, spb[st["eb"]][:, hh * 512 + cs:(hh + 1) * 512],
                             st["first"], True, [b_sp[st["eb"]]] + CONSTB, [xb_[hh]])

                def e_G(st):
                    cs = st["cs"]
                    k.act(V3(Gb[st["gb"]], cs), V3(XP[:, :], cs), AF.Exp, xb_, [b_G[st["gb"]]], scale=-1.0)

                def e_Tstr(st):
                    if st["last"]:
                        return
                    cs = st["cs"]
                    for hh in range(2):
                        k.mm(XP[:, hh * 512 + cs:(hh + 1) * 512], tstr16, spb[st["eb"]][:, hh * 512 + cs:(hh + 1) * 512],
                             False, True, [b_sp[st["eb"]]] + CONSTB, [xb_[hh]])

                def e_A(st):
                    cs = st["cs"]
                    k.tt("dve", V3(Ab[st["gb"]], cs), V3(Eb[st["eb"]], cs), V3(Gb[st["gb"]], cs), ALU.mult,
                         [b_E[st["eb"]], b_G[st["gb"]]], [b_A[st["gb"]]])

                def e_AV(st):
                    hp, kb, ob, cs = st["hp"], st["kb"], st["ob"], st["cs"]
                    for hh in range(2):
                        h = 2 * hp + hh
                        k.mm(banks[ob][hh * 64:(hh + 1) * 64, cs:512], vv[:, kb, h * 64:(h + 1) * 64],
                             Ab[st["gb"]][:, hh * 512 + cs:(hh + 1) * 512], st["first"], st["last"],
                             [b_vv[kb // 8], b_A[st["gb"]]], [bankb[ob]])
                    if st["last"]:
                        k.copy("dve", ub[:, hp, :], banks[ob][:, :], [bankb[ob]], [b_ub[hp]])

                mix_stride = max(1, N // 36)
                for i_ in range(-3, N):
                    if 0 <= i_ < N:
                        e_G(steps[i_])
                        e_Tstr(steps[i_])
                    if 0 <= i_ + 1 < N:
                        e_Tinc(steps[i_ + 1])
                    if 0 <= i_ < N:
                        e_A(steps[i_])
                        if dve_pending[0] is not None:
                            dve_pending[0]()
                            dve_pending[0] = None
                        if need_load[0] and not pend_mix:
                            load_mix_inputs()
                            need_load[0] = False
                    if 0 <= i_ + 2 < N:
                        e_E(steps[i_ + 2])
                    if 0 <= i_ + 3 < N:
                        e_Z(steps[i_ + 3])
                    if 0 <= i_ + 2 < N:
                        e_sp(steps[i_ + 2])
                    if 0 <= i_ < N:
                        e_AV(steps[i_])
                        if pend_mix and i_ % mix_stride == 0:
                            pe_, dv_ = pend_mix.pop(0)
                            pe_()
                            dve_pending[0] = dv_
                            if not pend_mix:
                                need_load[0] = True
                opair_i += 4

                if dve_pending[0] is not None:
                    dve_pending[0]()
                    dve_pending[0] = None
                if need_load[0]:
                    load_mix_inputs()
                    need_load[0] = False

                def mk_chunks(qi=qi, ub=ub, b_ub=b_ub):
                    ch = []
                    for oc in range(8):
                        r = oc % 2

                        def peA(oc=oc):
                            for kc in range(4):
                                k.mm(banks[6][:, :], wa[:, kc, oc * 128:(oc + 1) * 128], ua[:, kc, :], kc == 0, kc == 3,
                                     [b_wa, b_ua], [bankb[6]])

                        def dvA(oc=oc, r=r):
                            k.tt("dve", m1[r], banks[6][:, :], gt[:, oc, :], ALU.mult, [bankb[6], b_gt], [b_m1[r]])

                        def peB(oc=oc):
                            for kc in range(4):
                                k.mm(banks[7][:, :], wb[:, kc, oc * 128:(oc + 1) * 128], ub[:, kc, :], kc == 0, kc == 3,
                                     [b_wb, b_ub[kc]], [bankb[7]])

                        def dvB(oc=oc, r=r):
                            k.tt("dve", m2[r], banks[7][:, :], gt[:, 8 + oc, :], ALU.mult, [bankb[7], b_gt], [b_m2[r]])
                            k.tt("pool", mix[:, oc, :], m1[r], m2[r], ALU.add, [b_m1[r], b_m2[r]], [b_mix[oc]])
                        ch += [(peA, dvA), (peB, dvB)]
                    for oc in range(8):
                        bk = 6 + oc % 2

                        def peC(oc=oc, bk=bk):
                            for kc in range(4):
                                k.mm(banks[bk][:, :], wo[:, kc, oc * 128:(oc + 1) * 128], mix[:, kc, :], kc == 0, False,
                                     [b_wo, b_mix[kc]], [bankb[bk]])

                        def peD(oc=oc, bk=bk):
                            for kc in range(4, 8):
                                k.mm(banks[bk][:, :], wo[:, kc, oc * 128:(oc + 1) * 128], mix[:, kc, :], False, kc == 7,
                                     [b_wo, b_mix[kc]], [bankb[bk]])

                        def dvD(oc=oc, bk=bk):
                            k.tt("dve", xb[:, oc, :], xb[:, oc, :], banks[bk][:, :], ALU.add, [bankb[bk], b_xb[oc]], [b_xb[oc]])
                            if oc == 7:
                                k.dma("sp", xTs[qi], xb, b_xbd, reads=b_xb, writes=[b_xTs[qi]])
                        ch += [(peC, None), (peD, dvD)]
                    return ch

                assert not pend_mix
                pend_mix = mk_chunks()
            while pend_mix:
                pe_, dv_ = pend_mix.pop(0)
                pe_()
                if dv_ is not None:
                    dv_()
            k.barrier()
            if dbg == ("B", l):
                break

            moe = (l % 2 == 1)
            last = (l == DEPTH - 1)
            PT = 2048
            NPASS = S // PT
            TPP = PT // 512
            ar.off = PERS
            acc = ar.alloc([128, 8, PT], F32)
            h2 = ar.alloc([128, 8, PT], BF16)
            wg = [ar.alloc([128, 8, 512], BF16) for _ in range(2)]
            wu = [ar.alloc([128, 8, 512], BF16) for _ in range(2)]
            wd = [ar.alloc([128, 4, D], BF16) for _ in range(2)]
            hid = [ar.alloc([128, 4, 512], BF16) for _ in range(2)]
            sil = [ar.alloc([128, 512], F32) for _ in range(2)]
            sil2 = [ar.alloc([128, 512], F32) for _ in range(2)]
            cbb = [ar.alloc([128, 512], F32) for _ in range(4 if moe else 2)]
            h2f = [ar.alloc([128, 512], F32) for _ in range(2)]
            sqs = [ar.alloc([128, 512], BF16) for _ in range(2)]
            rstd = ar.alloc([128, 512], F32)
            combT = ar.alloc([128, PT], F32)
            wr = ar.alloc([128, 8, NE], F32)
            sm = [ar.alloc([128, 64], F32) for _ in range(2)]
            yf = ar.alloc([128, 8, 128], F32) if last else None
            ost = [ar.alloc([128, D], F32) for _ in range(2)] if last else None

            b_acc = k.bufs(8 * TPP, "acc")
            b_accd = k.bufs(TPP, "accd")
            b_h2 = k.bufs(8 * TPP, "h2")
            b_wg, b_wu, b_wd = k.bufs(2, "wg"), k.bufs(2, "wu"), k.bufs(2, "wd")
            b_hid = [k.bufs(4, "hid") for _ in range(2)]
            b_sil, b_sil2, b_cbb = k.bufs(2, "sil"), k.bufs(2, "sil2"), k.bufs(4, "cbb")
            b_h2f = k.bufs(2, "h2f")
            b_sq = k.bufs(2, "sq")
            b_rstd = k.buf("rstd")
            b_combT = k.bufs(TPP, "combT")
            b_wr = k.buf("wr")
            b_sm = k.bufs(2, "sm")
            b_yf = k.bufs(8, "yf")
            b_ost = k.bufs(2, "ost")

            if moe:
                k.dma("sp", wr, w_rt[0].rearrange("(kc p) e -> p kc e", p=128), b_wr, writes=[b_wr])
                units = [(e, off, 4) for e in range(NE) for off in range(0, DFE, 512)]
            else:
                units = [(0, off, 4) for off in range(0, 2560, 512)] + [(0, 2560, 2)]

            def load_unit(ui):
                e, off, nh = units[ui]
                p = ui % 2
                hc = nh * 128
                if moe:
                    gsrc, usrc, dsrc = w_eg[0, e], w_eu[0, e], w_ed[0, e]
                else:
                    gsrc, usrc, dsrc = w_fg[0], w_fu[0], w_fd[0]
                k.dma("pool", wg[p][:, :, 0:hc], gsrc.rearrange("(kc p) f -> p kc f", p=128)[:, :, off:off + hc], b_wg[p],
                      writes=[b_wg[p]])
                k.dma("pool", wu[p][:, :, 0:hc], usrc.rearrange("(kc p) f -> p kc f", p=128)[:, :, off:off + hc], b_wu[p],
                      writes=[b_wu[p]])
                k.dma("pool", wd[p][:, 0:nh, :], dsrc[off:off + hc, :].rearrange("(kc p) f -> p kc f", p=128), b_wd[p],
                      writes=[b_wd[p]])

            gcol = 16 + l * 8
            for ps_ in range(NPASS):
                for tl in range(TPP):
                    ti = ps_ * TPP + tl
                    k.dma("sp", acc[:, :, tl * 512:(tl + 1) * 512], xTs[ti], b_accd[tl], reads=[b_xTs[ti]],
                          writes=b_acc[tl * 8:(tl + 1) * 8])
                load_unit(0)

                def norm_tile(tl):
                    cols = slice(tl * 512, (tl + 1) * 512)
                    ab = b_acc[tl * 8:(tl + 1) * 8]
                    rmsnorm_stats([acc[:, c, cols] for c in range(8)], ab, sqs, b_sq, banks[7][:, :], bankb[7], rstd, b_rstd)
                    for c in range(8):
                        if moe:
                            r = c % 2
                            k.stt(h2f[r], acc[:, c, cols], vec[:, gcol + c:gcol + c + 1], rstd, ALU.mult, ALU.mult,
                                  [ab[c], b_rstd] + CONSTB, [b_h2f[r]])
                            k.copy("act", h2[:, c, cols], h2f[r], [b_h2f[r]], [b_h2[tl * 8 + c]])
                            for b in range(4):
                                k.mm(banks[b][:, 0:8], h2f[r][:, b * 128:(b + 1) * 128], wr[:, c, :], c == 0,
                                     c == 7, [b_h2f[r], b_wr], [bankb[b]])
                        else:
                            k.stt(h2[:, c, cols], acc[:, c, cols], vec[:, gcol + c:gcol + c + 1], rstd, ALU.mult, ALU.mult,
                                  [ab[c], b_rstd] + CONSTB, [b_h2[tl * 8 + c]])
                    if moe:
                        for b in range(4):
                            r = b % 2
                            t_ = sm[r]
                            lg, m8, dd, ee, w1, w2, c1, c2 = (t_[:, 0:8], t_[:, 8:16], t_[:, 16:17], t_[:, 17:18],
                                                              t_[:, 18:19], t_[:, 19:20], t_[:, 24:32], t_[:, 32:40])
                            bs = [b_sm[r]]
                            k.copy("dve", lg, banks[b][:, 0:8], [bankb[b]], bs)
                            k.op("dve", (lambda o_, i_: (lambda e: e.max(out=o_, in_=i_)))(m8, lg), bs, bs)
                            k.tt("dve", dd, m8[:, 1:2], m8[:, 0:1], ALU.subtract, bs, bs)
                            k.act(ee, dd, AF.Exp, bs, bs)
                            k.ts("dve", w1, ee, 1.0, None, ALU.add, None, bs, bs)
                            k.op("dve", (lambda o_: (lambda e: e.reciprocal(out=o_, in_=o_)))(w1), bs, bs)
                            k.tt("dve", w2, ee, w1, ALU.mult, bs, bs)
                            k.ts("dve", c1, lg, m8[:, 0:1], w1, ALU.is_equal, ALU.mult, bs, bs)
                            k.ts("dve", c2, lg, m8[:, 1:2], w2, ALU.is_equal, ALU.mult, bs, bs)
                            k.tt("dve", c1, c1, c2, ALU.add, bs, bs)
                            k.tr(banks[7][0:8, b * 128:(b + 1) * 128], c1, ident, bs + CONSTB, [bankb[7]])
                        k.copy("dve", combT[0:8, cols], banks[7][0:8, :], [bankb[7]], [b_combT[tl]])

                def fin_tile(tl, ps_=ps_):
                    ti = ps_ * TPP + tl
                    cols = slice(tl * 512, (tl + 1) * 512)
                    ab = b_acc[tl * 8:(tl + 1) * 8]
                    if not last:
                        k.dma("sp", xTs[ti], acc[:, :, cols], b_accd[tl], reads=ab, writes=[b_xTs[ti]])
                    else:
                        rmsnorm_stats([acc[:, c, cols] for c in range(8)], ab, sqs, b_sq, banks[7][:, :], bankb[7], rstd,
                                      b_rstd)
                        for b in range(4):
                            r = b % 2
                            bc = slice(tl * 512 + b * 128, tl * 512 + (b + 1) * 128)
                            for c in range(8):
                                k.stt(yf[:, c, :], acc[:, c, bc], vec[:, 32 + c:33 + c], rstd[:, b * 128:(b + 1) * 128],
                                      ALU.mult, ALU.mult, [ab[c], b_rstd] + CONSTB, [b_yf[c]])
                            for c in range(8):
                                bk = 5 + c // 4
                                k.tr(banks[bk][:, (c % 4) * 128:(c % 4 + 1) * 128], yf[:, c, :], ident,
                                     [b_yf[c]] + CONSTB, [bankb[bk]])
                            k.copy("act", ost[r][:, 0:512], banks[5][:, :], [bankb[5]], [b_ost[r]])
                            k.copy("dve", ost[r][:, 512:1024], banks[6][:, :], [bankb[6]], [b_ost[r]])
                            t0 = ti * 512 + b * 128
                            k.dma("sp", out[t0:t0 + 128, :], ost[r], b_ost[r], reads=[b_ost[r]])

                pend = []
                pdi = [0]
                it = 0
                for ui in range(len(units)):
                    e_, off, nh = units[ui]
                    p = ui % 2
                    for tl in range(TPP):
                        cols = slice(tl * 512, (tl + 1) * 512)
                        hp_ = it % 2
                        it += 1
                        if ui == 0:
                            norm_tile(tl)
                        if moe:
                            if off == 0:
                                k.mm(banks[7][:, :], sel[0:8, e_ * 128:(e_ + 1) * 128], combT[0:8, cols], True, True,
                                     [b_combT[tl]] + CONSTB, [bankb[7]])
                                k.copy("act", cbb[tl], banks[7][:, :], [bankb[7]], [b_cbb[tl]])
                        for hc in range(nh):
                            r = hc % 2
                            for kc in range(8):
                                k.mm(banks[r][:, :], wg[p][:, kc, hc * 128:(hc + 1) * 128], h2[:, kc, cols], kc == 0, kc == 7,
                                     [b_wg[p], b_h2[tl * 8 + kc]], [bankb[r]])
                            for kc in range(8):
                                k.mm(banks[2 + r][:, :], wu[p][:, kc, hc * 128:(hc + 1) * 128], h2[:, kc, cols], kc == 0,
                                     kc == 7, [b_wu[p], b_h2[tl * 8 + kc]], [bankb[2 + r]])
                            k.act(sil[r], banks[r][:, :], AF.Silu, [bankb[r]], [b_sil[r]])
                            if moe:
                                k.tt("pool", sil2[r], sil[r], cbb[tl], ALU.mult, [b_sil[r], b_cbb[tl]], [b_sil2[r]])
                                k.tt("dve", hid[hp_][:, hc, :], sil2[r], banks[2 + r][:, :], ALU.mult,
                                     [b_sil2[r], bankb[2 + r]], [b_hid[hp_][hc]])
                            else:
                                k.tt("dve", hid[hp_][:, hc, :], sil[r], banks[2 + r][:, :], ALU.mult,
                                     [b_sil[r], bankb[2 + r]], [b_hid[hp_][hc]])
                            for _ in range(8 // nh):
                                if pend:
                                    pend.pop(0)()

                        def mk_down(oc, p=p, nh=nh, hp_=hp_, tl=tl, cols=cols):
                            def f_():
                                bk = 4 + pdi[0] % 3
                                pdi[0] += 1
                                for hc in range(nh):
                                    k.mm(banks[bk][:, :], wd[p][:, hc, oc * 128:(oc + 1) * 128], hid[hp_][:, hc, :], hc == 0,
                                         hc == nh - 1, [b_wd[p], b_hid[hp_][hc]], [bankb[bk]])
                                k.tt("dve", acc[:, oc, cols], acc[:, oc, cols], banks[bk][:, :], ALU.add,
                                     [bankb[bk], b_acc[tl * 8 + oc]], [b_acc[tl * 8 + oc]])
                            return f_
                        while pend:
                            pend.pop(0)()
                        pend.extend(mk_down(oc) for oc in range(8))
                        if tl == 0 and ui + 1 < len(units):
                            load_unit(ui + 1)
                        if ui == len(units) - 1 and tl >= 1:
                            fin_tile(tl - 1)
                while pend:
                    pend.pop(0)()

                fin_tile(TPP - 1)
            k.barrier()
            if dbg == ("C", l):
                break

        k.barrier()
        k.replay()
    return nc


def _consts():
    c = np.zeros((128, C_END), np.float32)
    c[:, C_ID:C_ID + 128] = np.eye(128, dtype=np.float32)
    j = np.arange(128)[:, None]
    s_ = np.arange(128)[None, :]
    c[:, C_TINC:C_TINC + 128] = (j >= s_)
    c[:, C_TSTR:C_TSTR + 128] = (j < s_)
    c[:, C_ONES:C_ONES + 128] = 1.0
    t = np.arange(512)[None, :]
    for jj in range(4):
        c[:, C_MASK + jj * 512:C_MASK + (jj + 1) * 512] = (t > 128 * jj + j)
    for e in range(NE):
        c[e, C_SEL + e * 128:C_SEL + (e + 1) * 128] = 1.0
    return c


def _vecs(g_mix, g_ffn, g_final, conv_w):
    v = np.zeros((128, 64), np.float32)
    for l in range(DEPTH):
        v[:, l * 8:(l + 1) * 8] = np.asarray(g_mix[l]).reshape(8, 128).T
        v[:, 16 + l * 8:16 + (l + 1) * 8] = np.asarray(g_ffn[l]).reshape(8, 128).T
        for kk in range(3):
            v[:, 40 + l * 12 + kk * 4:40 + l * 12 + kk * 4 + 4] = np.asarray(conv_w[l, kk]).reshape(4, 128).T
    v[:, 32:40] = np.asarray(g_final).reshape(8, 128).T
    return v


_NC_CACHE = {}


def kernel(x, g_mix, w_in, conv_w, w_branch_conv, w_branch_attn, w_out, g_ffn, w_ffn_gate, w_ffn_up, w_ffn_down,
           w_router, w_exp_gate, w_exp_up, w_exp_down, g_final):
    f = lambda a: np.ascontiguousarray(np.asarray(a, dtype=np.float32))
    x = f(x)
    shared = {
        "vecs": _vecs(f(g_mix), f(g_ffn), f(g_final), f(conv_w)),
        "consts": _consts(),
        "w_in": f(w_in), "w_branch_conv": f(w_branch_conv), "w_branch_attn": f(w_branch_attn), "w_out": f(w_out),
        "w_ffn_gate": f(w_ffn_gate), "w_ffn_up": f(w_ffn_up), "w_ffn_down": f(w_ffn_down), "w_router": f(w_router),
        "w_exp_gate": f(w_exp_gate), "w_exp_up": f(w_exp_up), "w_exp_down": f(w_exp_down),
    }
    if "nc" not in _NC_CACHE:
        _NC_CACHE["nc"] = build()
    nc = _NC_CACHE["nc"]
    in_maps = []
    for i in range(8):
        m = dict(shared)
        m["x"] = x[i]
        in_maps.append(m)
    res = run_bass_kernel_spmd(nc, in_maps, core_ids=list(range(8)))
    return np.stack([r["out"] for r in res.results], axis=0).astype(np.float32)
```
